# Optimizing a Trainium2 kernel written in Bass

```python
import jax
import jax.numpy as jnp
from jax import lax
import numpy as np

D_MODEL = 2048
BATCH = 4
SEQ = 4096
DEPTH = 2

GRID_W = 64
CTX_LEN = 256
RMS_EPS = 1e-6

HG_HEADS = 8
HG_DK = 128
HG_DV = 128
HG_WIDTH = HG_HEADS * HG_DV
HG_CHUNK = 64
POOL_WINDOWS = (2, 4, 8, 16)
POOL_WIDTH = 1024
POOL_GROUP = POOL_WIDTH // len(POOL_WINDOWS)
MLA_HEADS = 16
Q_LORA = 512
KV_LORA = 512
QK_NOPE = 128
QK_ROPE = 64
V_HEAD = 128
MLA_WIDTH = MLA_HEADS * V_HEAD
MLA_SCALE = (QK_NOPE + QK_ROPE) ** -0.5
ROPE_THETA = 10000.0
Q_BLOCK = 128
N_BRANCH = 3
PEER_HEADS = 8
PEER_NKEYS = 128
PEER_N = PEER_NKEYS * PEER_NKEYS
PEER_DKEY = 256
PEER_TOPK = 16
TOK_BLOCK = 128

IN_SIZES = (HG_HEADS * HG_DK, HG_HEADS * HG_DK, HG_HEADS * HG_DK, HG_WIDTH, HG_WIDTH,
            POOL_WIDTH, Q_LORA, KV_LORA, QK_ROPE, N_BRANCH * D_MODEL)
IN_COLS = sum(IN_SIZES)

kernel_name = 'hybrid_hgrn2_pool_mla_peer_dit_block'


def rms_norm(x, w, eps=RMS_EPS):
    xf = x.astype(jnp.float32)
    y = xf * lax.rsqrt(jnp.mean(xf * xf, axis=-1, keepdims=True) + eps)
    return (y * w.astype(jnp.float32)).astype(x.dtype)


def modulate(x, gain, shift, scale):
    return rms_norm(x, gain) * (1 + scale) + shift


def split_projection(p):
    parts, start = [], 0
    for size in IN_SIZES:
        parts.append(p[..., start:start + size])
        start += size
    return parts


def axial_rope_tables(n_tokens):
    rows = n_tokens // GRID_W
    r, col = jnp.meshgrid(jnp.arange(rows), jnp.arange(GRID_W), indexing='ij')
    n_freq = QK_ROPE // 4
    freqs = ROPE_THETA ** (-jnp.arange(n_freq, dtype=jnp.float32) / n_freq)
    ang = jnp.stack([r.reshape(-1)[:, None] * freqs, col.reshape(-1)[:, None] * freqs], axis=1)
    return jnp.cos(ang), jnp.sin(ang)


def apply_axial_rope(x, cos, sin):
    xs = x.reshape(x.shape[:-1] + (2, 2, QK_ROPE // 4))
    cs = cos[None, :, None].astype(x.dtype)
    sn = sin[None, :, None].astype(x.dtype)
    x1, x2 = xs[..., 0, :], xs[..., 1, :]
    return jnp.stack([x1 * cs - x2 * sn, x2 * cs + x1 * sn], axis=-2).reshape(x.shape)


def hgrn_lower_bounds(logits):
    cum = jnp.cumsum(jax.nn.softmax(logits.astype(jnp.float32), axis=0), axis=0)
    return cum - cum[0:1]


def gla_chunkwise(q, k, v, log_f, s0):
    B_, H_, L, _ = q.shape
    n = L // HG_CHUNK

    def to_chunks(t):
        return jnp.moveaxis(t.reshape(B_, H_, n, HG_CHUNK, t.shape[-1]), 2, 0)

    causal = jnp.tril(jnp.ones((HG_CHUNK, HG_CHUNK), dtype=bool))

    def step(S, chunk):
        qc, kc, vc, lf = chunk
        b = jnp.cumsum(lf, axis=2)
        b_end = b[:, :, -1:, :]
        rel = jnp.where(causal[:, :, None], b[:, :, :, None, :] - b[:, :, None, :, :], -jnp.inf)
        scores = jnp.einsum('bhtc,bhsc,bhtsc->bhts', qc, kc, jnp.exp(rel))
        o = (jnp.einsum('bhts,bhsv->bhtv', scores, vc)
             + jnp.einsum('bhtc,bhcv->bhtv', qc * jnp.exp(b), S))
        S_new = (jnp.exp(b_end[:, :, 0, :])[..., None] * S
                 + jnp.einsum('bhsc,bhsv->bhcv', kc * jnp.exp(b_end - b), vc))
        return S_new, o

    s_final, o = lax.scan(step, s0, (to_chunks(q), to_chunks(k), to_chunks(v), to_chunks(log_f)))
    return jnp.moveaxis(o, 0, 2).reshape(B_, H_, L, v.shape[-1]), s_final


def hgrn_direction(q, f_raw, v, lb, s0, reverse):
    lbf = lb.astype(jnp.float32)
    log_f = jnp.logaddexp(jnp.log(lbf), jnp.log1p(-lbf) + jax.nn.log_sigmoid(f_raw.astype(jnp.float32)))
    k = -jnp.expm1(log_f)

    def heads(t, d):
        t = t.astype(jnp.float32)
        if reverse:
            t = jnp.flip(t, axis=1)
        return jnp.transpose(t.reshape(t.shape[0], t.shape[1], HG_HEADS, d), (0, 2, 1, 3))

    o, s_final = gla_chunkwise(heads(q, HG_DK), heads(k, HG_DK), heads(v, HG_DV), heads(log_f, HG_DK), s0)
    if reverse:
        o = jnp.flip(o, axis=2)
    return o, s_final


def hgrn_readout(o, g, norm_w):
    B_, _, L, _ = o.shape
    o = rms_norm(jnp.transpose(o, (0, 2, 1, 3)), norm_w).reshape(B_, L, HG_WIDTH)
    return (o * jax.nn.silu(g.astype(jnp.float32))).astype(g.dtype)


def hgrn2_mixer(pc, pl, lb_pair, norm_w, need_ctx_out):
    s0 = jnp.zeros((pl[0].shape[0], HG_HEADS, HG_DK, HG_DV), jnp.float32)
    qc, ql = jax.nn.silu(pc[0]), jax.nn.silu(pl[0])
    oc_f, s_fwd = hgrn_direction(qc, pc[1], pc[3], lb_pair[0], s0, False)
    oc_b, s_bwd = hgrn_direction(qc, pc[2], pc[3], lb_pair[1], s0, True)
    ol_f, _ = hgrn_direction(ql, pl[1], pl[3], lb_pair[0], s_fwd, False)
    ol_b, _ = hgrn_direction(ql, pl[2], pl[3], lb_pair[1], s_bwd, True)
    out_l = hgrn_readout(ol_f + ol_b, pl[4], norm_w)
    out_c = hgrn_readout(oc_f + oc_b, pc[4], norm_w) if need_ctx_out else None
    return out_l, out_c


def multiscale_pool(u, w_pool, pool_scale):
    B_, L, _ = u.shape
    uf = u.astype(jnp.float32)
    csum = jnp.concatenate([jnp.zeros((B_, 1, POOL_WIDTH), jnp.float32), jnp.cumsum(uf, axis=1)], axis=1)
    t = jnp.arange(L)
    groups = []
    for gi, win in enumerate(POOL_WINDOWS):
        lo = jnp.clip(t - win // 2, 0, L)
        hi = jnp.clip(t + win // 2, 0, L)
        sl = slice(gi * POOL_GROUP, (gi + 1) * POOL_GROUP)
        cs = csum[:, :, sl]
        mean = (cs[:, hi] - cs[:, lo]) / (hi - lo).astype(jnp.float32)[None, :, None]
        groups.append(mean - uf[:, :, sl])
    pooled = jnp.stack(groups, axis=2)
    mixed = jnp.einsum('blgi,gio->blgo', pooled, w_pool.astype(jnp.float32))
    return (mixed.reshape(B_, L, POOL_WIDTH) * pool_scale.astype(jnp.float32)).astype(u.dtype)


def mla_queries(c_q, q_norm_w, w_uq, rope):
    B_, L, _ = c_q.shape
    q = (rms_norm(c_q, q_norm_w) @ w_uq).reshape(B_, L, MLA_HEADS, QK_NOPE + QK_ROPE)
    if rope is None:
        return q
    return jnp.concatenate([q[..., :QK_NOPE], apply_axial_rope(q[..., QK_NOPE:], *rope)], axis=-1)


def mla_keys_values(c_kv, k_rope_raw, kv_norm_w, w_ukv, rope):
    B_, L, _ = c_kv.shape
    kv = (rms_norm(c_kv, kv_norm_w) @ w_ukv).reshape(B_, L, MLA_HEADS, QK_NOPE + V_HEAD)
    k_rope = k_rope_raw[:, :, None, :]
    if rope is not None:
        k_rope = apply_axial_rope(k_rope, *rope)
    k = jnp.concatenate([kv[..., :QK_NOPE], jnp.broadcast_to(k_rope, (B_, L, MLA_HEADS, QK_ROPE))], axis=-1)
    return k, kv[..., QK_NOPE:]


def block_attention(q, k, v):
    B_, L, H_, Dqk = q.shape
    nb = L // Q_BLOCK
    q_blocks = jnp.moveaxis(q.reshape(B_, nb, Q_BLOCK, H_, Dqk), 1, 0)

    def attend(qb):
        s = jnp.einsum('bqhd,bkhd->bhqk', qb, k).astype(jnp.float32) * MLA_SCALE
        p = jax.nn.softmax(s, axis=-1).astype(v.dtype)
        return jnp.einsum('bhqk,bkhd->bqhd', p, v)

    o = lax.map(attend, q_blocks)
    return jnp.moveaxis(o, 0, 1).reshape(B_, L, H_ * v.shape[-1])


def mla_mixer(pc, pl, q_norm_w, w_uq, kv_norm_w, w_ukv, rope, need_ctx_out):
    k_c, v_c = mla_keys_values(pc[1], pc[2], kv_norm_w, w_ukv, None)
    k_l, v_l = mla_keys_values(pl[1], pl[2], kv_norm_w, w_ukv, rope)
    q_l = mla_queries(pl[0], q_norm_w, w_uq, rope)
    out_l = block_attention(q_l, jnp.concatenate([k_c, k_l], axis=1), jnp.concatenate([v_c, v_l], axis=1))
    out_c = block_attention(mla_queries(pc[0], q_norm_w, w_uq, None), k_c, v_c) if need_ctx_out else None
    return out_l, out_c


def merge_branches(y_a, y_b, y_c, gate_raw, w_a, w_b, w_c, w_o):
    g = jax.nn.sigmoid(gate_raw)
    m = (g[..., :D_MODEL] * (y_a @ w_a)
         + g[..., D_MODEL:2 * D_MODEL] * (y_b @ w_b)
         + g[..., 2 * D_MODEL:] * (y_c @ w_c))
    return m @ w_o


def peer_ffn(h, wq, keys, u_tab, v_tab):
    B_, L, D = h.shape
    tokens = h.reshape(-1, TOK_BLOCK, D)

    def one_block(hb):
        T = hb.shape[0]
        qh = (hb @ wq).reshape(T, PEER_HEADS, 2, PEER_DKEY // 2)
        s = jnp.einsum('thpd,phkd->thpk', qh, keys).astype(jnp.float32)
        s_top, i_top = lax.top_k(s, PEER_TOPK)
        cand = (s_top[:, :, 0, :, None] + s_top[:, :, 1, None, :]).reshape(T, PEER_HEADS, -1)
        cand_idx = (i_top[:, :, 0, :, None] * PEER_NKEYS + i_top[:, :, 1, None, :]).reshape(T, PEER_HEADS, -1)
        best, pos = lax.top_k(cand, PEER_TOPK)
        idx = jnp.take_along_axis(cand_idx, pos, axis=-1)
        gate = jax.nn.softmax(best, axis=-1)
        u = jnp.take(u_tab, idx, axis=0)
        act = jnp.einsum('thkd,td->thk', u, hb).astype(jnp.float32)
        w = (gate * jax.nn.gelu(act, approximate=False)).astype(v_tab.dtype)
        return jnp.einsum('thk,thkd->td', w, jnp.take(v_tab, idx, axis=0))

    return lax.map(one_block, tokens).reshape(B_, L, D)


def setup_inputs(seed: int = 0) -> dict:
    key = jax.random.key(seed)
    ks = jax.random.split(key, 32)
    D = D_MODEL

    def nrm(i, shape, std):
        return jax.random.normal(ks[i], shape, jnp.float32) * std

    def gain(i, shape):
        return 1.0 + nrm(i, shape, 0.05)

    return {
        'x': nrm(0, (BATCH, SEQ, D), 1.0),
        'c': nrm(1, (BATCH, D), 1.0),
        'ctx': nrm(2, (BATCH, CTX_LEN, D), 1.0),
        'c_ctx': nrm(3, (D,), 1.0),
        'w_mod': nrm(4, (DEPTH, D, 6 * D), 0.5 * D ** -0.5),
        'b_mod': nrm(5, (DEPTH, 6 * D), 0.01),
        'norm_mix': gain(6, (DEPTH, D)),
        'norm_ffn': gain(7, (DEPTH, D)),
        'w_in': nrm(8, (DEPTH, D, IN_COLS), D ** -0.5),
        'hg_lb_logits': nrm(9, (DEPTH, 2, HG_HEADS * HG_DK), 1.0),
        'hg_norm': gain(10, (DEPTH, HG_DV)),
        'pool_w': nrm(11, (DEPTH, len(POOL_WINDOWS), POOL_GROUP, POOL_GROUP), POOL_GROUP ** -0.5),
        'pool_scale': gain(12, (DEPTH, POOL_WIDTH)),
        'mla_q_norm': gain(13, (DEPTH, Q_LORA)),
        'mla_w_uq': nrm(14, (DEPTH, Q_LORA, MLA_HEADS * (QK_NOPE + QK_ROPE)), Q_LORA ** -0.5),
        'mla_kv_norm': gain(15, (DEPTH, KV_LORA)),
        'mla_w_ukv': nrm(16, (DEPTH, KV_LORA, MLA_HEADS * (QK_NOPE + V_HEAD)), KV_LORA ** -0.5),
        'w_branch_a': nrm(17, (DEPTH, HG_WIDTH, D), HG_WIDTH ** -0.5),
        'w_branch_b': nrm(18, (DEPTH, POOL_WIDTH, D), POOL_WIDTH ** -0.5),
        'w_branch_c': nrm(19, (DEPTH, MLA_WIDTH, D), MLA_WIDTH ** -0.5),
        'w_out': nrm(20, (DEPTH, D, D), D ** -0.5),
        'peer_wq': nrm(21, (DEPTH, D, PEER_HEADS * PEER_DKEY), D ** -0.5),
        'peer_keys': nrm(22, (DEPTH, 2, PEER_HEADS, PEER_NKEYS, PEER_DKEY // 2), (PEER_DKEY // 2) ** -0.5),
        'peer_u': nrm(23, (DEPTH, PEER_N, D), D ** -0.5),
        'peer_v': nrm(24, (DEPTH, PEER_N, D), PEER_HEADS ** -0.5),
        'final_norm': gain(25, (D,)),
    }


def reference(x, c, ctx, c_ctx, w_mod, b_mod, norm_mix, norm_ffn, w_in, hg_lb_logits, hg_norm,
              pool_w, pool_scale, mla_q_norm, mla_w_uq, mla_kv_norm, mla_w_ukv,
              w_branch_a, w_branch_b, w_branch_c, w_out, peer_wq, peer_keys, peer_u, peer_v, final_norm):
    rope = axial_rope_tables(x.shape[1])
    lower_bounds = hgrn_lower_bounds(hg_lb_logits)
    xc = ctx
    for l in range(DEPTH):
        last = l == DEPTH - 1
        mod_l = jnp.split((jax.nn.silu(c) @ w_mod[l] + b_mod[l])[:, None, :], 6, axis=-1)
        mod_c = jnp.split((jax.nn.silu(c_ctx) @ w_mod[l] + b_mod[l])[None, None, :], 6, axis=-1)
        h_l = modulate(x, norm_mix[l], mod_l[0], mod_l[1])
        h_c = modulate(xc, norm_mix[l], mod_c[0], mod_c[1])
        p_l = split_projection(h_l @ w_in[l])
        p_c = split_projection(h_c @ w_in[l])
        hg_l, hg_c = hgrn2_mixer(p_c[0:5], p_l[0:5], lower_bounds[l], hg_norm[l], not last)
        att_l, att_c = mla_mixer(p_c[6:9], p_l[6:9], mla_q_norm[l], mla_w_uq[l], mla_kv_norm[l],
                                 mla_w_ukv[l], rope, not last)
        pool_l = multiscale_pool(p_l[5], pool_w[l], pool_scale[l])
        x = x + mod_l[2] * merge_branches(hg_l, pool_l, att_l, p_l[9],
                                          w_branch_a[l], w_branch_b[l], w_branch_c[l], w_out[l])
        h2_l = modulate(x, norm_ffn[l], mod_l[3], mod_l[4])
        x = x + mod_l[5] * peer_ffn(h2_l, peer_wq[l], peer_keys[l], peer_u[l], peer_v[l])
        if not last:
            pool_c = multiscale_pool(p_c[5], pool_w[l], pool_scale[l])
            xc = xc + mod_c[2] * merge_branches(hg_c, pool_c, att_c, p_c[9],
                                                w_branch_a[l], w_branch_b[l], w_branch_c[l], w_out[l])
            h2_c = modulate(xc, norm_ffn[l], mod_c[3], mod_c[4])
            xc = xc + mod_c[5] * peer_ffn(h2_c, peer_wq[l], peer_keys[l], peer_u[l], peer_v[l])
    return rms_norm(x, final_norm)
```

```python
import numpy as np
import concourse.bass as bass
import concourse.mybir as mybir
from concourse.bass_utils import run_bass_kernel_spmd

F32 = mybir.dt.float32
BF16 = mybir.dt.bfloat16
U32 = mybir.dt.uint32
AF = mybir.ActivationFunctionType
ALU = mybir.AluOpType
AX = mybir.AxisListType

D = 2048
NCORE = 8
B = 4
SEQ = 4096
CTX = 256
LTOT = SEQ + CTX
T_CORE = 2048 + 128
IN_COLS = 13376
EPS = 1e-6


class KB:
    SEM_ROLL = 30000
    NDMA = 24

    def __init__(self):
        self.nc = bass.Bass("TRN2", target_bir_lowering=False)
        nc = self.nc
        self.eng = {"pe": nc.tensor, "dve": nc.vector, "act": nc.scalar, "pool": nc.gpsimd, "sp": nc.sync}
        self._keep = []
        self.nsem = 0
        self.sem = {}
        self.cnt = {}
        for e in ("pe", "dve", "act", "pool"):
            self._new_sem(e)
        self.dma_sems = [self._mk_sem() for _ in range(self.NDMA)]
        self.dma_val = [0] * self.NDMA
        self.dma_n = 0
        self.waited = {e: {} for e in self.eng}
        self.last_w = {}
        self.readers = {}
        self.ntens = 0

    def _mk_sem(self):
        self.nsem += 1
        cm = self.nc.semaphore(f"s{self.nsem}")
        s = cm.__enter__()
        self._keep.append(cm)
        return s

    def _new_sem(self, e):
        self.sem[e] = self._mk_sem()
        self.cnt[e] = 0

    def sb(self, shape, dt=F32, name=None):
        self.ntens += 1
        cm = self.nc.sbuf_tensor(self._nm(name) or f"t{self.ntens}", list(shape), dt)
        t = cm.__enter__()
        self._keep.append(cm)
        return t

    def ps(self, shape, dt=F32, name=None):
        self.ntens += 1
        cm = self.nc.psum_tensor(self._nm(name) or f"p{self.ntens}", list(shape), dt)
        t = cm.__enter__()
        self._keep.append(cm)
        return t

    def dram(self, name, shape, dt=F32, kind="ExternalInput"):
        return self.nc.dram_tensor(name, list(shape), dt, kind=kind).ap()

    def _nm(self, name):
        return None if name is None else "sb_" + name

    def _wait(self, e, ev):
        sem, val = ev
        k = id(sem)
        w = self.waited[e]
        if w.get(k, 0) >= val:
            return
        w[k] = val
        self.eng[e].wait_ge(sem, val)

    def _deps(self, e, r, w):
        evs = []
        for b in list(r) + list(w):
            ev = self.last_w.get(b)
            if ev is not None:
                evs.append(ev)
        for b in w:
            for ev in self.readers.get(b, {}).values():
                evs.append(ev)
        for ev in evs:
            if e == "pe" and ev[0] is self.sem.get("pe"):
                continue
            self._wait(e, ev)

    def _mark(self, ev, r, w):
        for b in w:
            self.last_w[b] = ev
            self.readers[b] = {}
        for b in r:
            self.readers.setdefault(b, {})[id(ev[0])] = ev

    def op(self, e, fn, r=(), w=()):
        self._deps(e, r, w)
        ins = fn(self.eng[e])
        if self.cnt[e] >= self.SEM_ROLL:
            self._new_sem(e)
        self.cnt[e] += 1
        ins.then_inc(self.sem[e], 1)
        ev = (self.sem[e], self.cnt[e])
        self._mark(ev, r, w)
        return ev

    def dma(self, q, out, in_, r=(), w=()):
        self._deps(q, r, w)
        s = self.dma_n % self.NDMA
        self.dma_n += 1
        if self.dma_val[s] > 0:
            self._wait(q, (self.dma_sems[s], self.dma_val[s]))
        self.eng[q].dma_start(out=out, in_=in_).then_inc(self.dma_sems[s], 16)
        self.dma_val[s] += 16
        ev = (self.dma_sems[s], self.dma_val[s])
        self._mark(ev, r, w)
        return ev

    def finish(self, outs):
        for b in outs:
            ev = self.last_w.get(b)
            if ev is not None:
                self._wait("sp", ev)
        for s in range(self.NDMA):
            if self.dma_val[s] > 0:
                self._wait("sp", (self.dma_sems[s], self.dma_val[s]))
        return self.nc


def run(kb, in_maps):
    res = run_bass_kernel_spmd(kb.nc, in_maps, core_ids=list(range(NCORE)))
    return res.results


def build_mod():
    kb = KB()
    NCOL = 3072
    cT = kb.dram("cT", [128, 16, 8])
    w = kb.dram("w", [D, NCOL])
    bb = kb.dram("bb", [8, NCOL])
    out = kb.dram("out", [8, NCOL], kind="ExternalOutput")
    ct = kb.sb([128, 16, 8])
    st = kb.sb([128, 16, 8])
    bt = kb.sb([8, NCOL])
    ot = kb.sb([8, NCOL])
    kb.dma("sp", ct[:], cT, w=["ct"])
    kb.dma("sp", bt[:], bb, w=["bt"])
    kb.op("act", lambda e: e.activation(out=st[:], in_=ct[:], func=AF.Silu), r=["ct"], w=["st"])
    wv = w.rearrange("(kc p) n -> p kc n", p=128)
    wts = [kb.sb([128, 16, 512], name=f"w{i}") for i in range(2)]
    pss = [kb.ps([8, 512], name=f"ps{i}") for i in range(2)]
    for g in range(NCOL // 512):
        wt = wts[g % 2]
        kb.dma("sp", wt[:], wv[:, :, g * 512:(g + 1) * 512], w=[("w", g % 2)])
        ps = pss[g % 2]
        for kc in range(16):
            kb.op("pe", lambda e: e.matmul(ps[:], lhsT=st[:, kc, :], rhs=wt[:, kc, :], start=(kc == 0), stop=(kc == 15)),
                  r=["st", ("w", g % 2)], w=[("ps", g % 2)])
        kb.op("dve", lambda e: e.tensor_tensor(out=ot[:, g * 512:(g + 1) * 512], in0=ps[:], in1=bt[:, g * 512:(g + 1) * 512], op=ALU.add),
              r=[("ps", g % 2), "bt"], w=["ot"])
    kb.dma("sp", out, ot[:], r=["ot"], w=["out"])
    kb.finish(["out"])
    return kb


def run_mod(inp):
    c, c_ctx, w_mod, b_mod = inp["c"], inp["c_ctx"], inp["w_mod"], inp["b_mod"]
    cs = np.zeros((8, D), np.float32)
    cs[:4] = c
    cs[4] = c_ctx
    cT = np.ascontiguousarray(cs.T.reshape(16, 128, 8).transpose(1, 0, 2))
    wall = np.concatenate([w_mod[0], w_mod[1]], axis=1)
    ball = np.concatenate([b_mod[0], b_mod[1]], axis=0)
    kb = build_mod()
    maps = []
    for i in range(NCORE):
        sl = slice(i * 3072, (i + 1) * 3072)
        maps.append({"cT": cT, "w": np.ascontiguousarray(wall[:, sl]),
                     "bb": np.ascontiguousarray(np.broadcast_to(ball[sl], (8, 3072)))})
    res = run(kb, maps)
    mod = np.concatenate([r["out"] for r in res], axis=1)
    return mod[:5].reshape(5, 2, 6, D)


NW = IN_COLS + 64
TGROUPS = [(0, 512), (512, 512), (1024, 512), (1536, 512), (2048, 128)]


def emit_norm_mod(kb, xT_dram, hT, ones_b, g1l, shl, g1c, shc, tag, x_keep=None):
    xv = xT_dram.rearrange("(kc p) t -> p kc t", p=128)
    xts = [kb.sb([128, 16, 512], name=f"{tag}x{i}") for i in range(1)] if x_keep is None else None
    sq = kb.sb([128, 16, 512], BF16, name=f"{tag}sq")
    tmp = xts[0] if x_keep is None else kb.sb([128, 16, 512], name=f"{tag}tmp")
    rs = kb.sb([128, 512], name=f"{tag}rs")
    pss = kb.ps([128, 512], name=f"{tag}pss")
    for gi, (t0, n) in enumerate(TGROUPS):
        if x_keep is None:
            xt = xts[0]
            xa = xt[:, :, 0:n]
            xk = tag + "tmp"
            kb.dma("sp", xa, xv[:, :, t0:t0 + n], w=[xk])
        else:
            xa = x_keep[:, :, t0:t0 + n]
            xk = (tag + "xk", gi)
            kb.dma("sp", xa, xv[:, :, t0:t0 + n], w=[xk])
        kb.op("act", lambda e: e.activation(out=sq[:, :, 0:n], in_=xa, func=AF.Square), r=[xk], w=[tag + "sq"])
        for kc in range(16):
            kb.op("pe", lambda e: e.matmul(pss[:, 0:n], lhsT=ones_b[:], rhs=sq[:, kc, 0:n], start=(kc == 0), stop=(kc == 15)),
                  r=[tag + "sq"], w=[tag + "pss"])
        kb.op("dve", lambda e: e.tensor_scalar(out=rs[:, 0:n], in0=pss[:, 0:n], scalar1=1.0 / D, scalar2=EPS, op0=ALU.mult, op1=ALU.add),
              r=[tag + "pss"], w=[tag + "rs"])
        kb.op("act", lambda e: e.activation(out=rs[:, 0:n], in_=rs[:, 0:n], func=AF.Sqrt), r=[tag + "rs"], w=[tag + "rs"])
        kb.op("dve", lambda e: e.reciprocal(out=rs[:, 0:n], in_=rs[:, 0:n]), r=[tag + "rs"], w=[tag + "rs"])
        kb.op("dve", lambda e: e.tensor_tensor(out=tmp[:, :, 0:n], in0=xa, in1=rs[:, 0:n].unsqueeze(1).broadcast_to([128, 16, n]), op=ALU.mult),
              r=[xk, tag + "rs"], w=[tag + "tmp"])
        g1, sh = (g1l, shl) if t0 < 2048 else (g1c, shc)
        for kc in range(16):
            kb.op("act", lambda e: e.activation(out=hT[:, kc, t0:t0 + n], in_=tmp[:, kc, 0:n], func=AF.Identity,
                                                scale=g1[:, kc:kc + 1], bias=sh[:, kc:kc + 1]),
                  r=[tag + "tmp", tag + "g"], w=[(tag + "hT", gi)])


def emit_mod_vectors(kb, nw_d, mv_d, tag):
    nw = kb.sb([128, 16], name=tag + "nw")
    mv = kb.sb([128, 16, 4], name=tag + "mv")
    g1l = kb.sb([128, 16], name=tag + "g1l")
    g1c = kb.sb([128, 16], name=tag + "g1c")
    shl = kb.sb([128, 16], name=tag + "shl")
    shc = kb.sb([128, 16], name=tag + "shc")
    kb.dma("sp", nw[:], nw_d, w=[tag + "nw"])
    kb.dma("sp", mv[:], mv_d, w=[tag + "mv"])
    for (g1, sh, o) in ((g1l, shl, 0), (g1c, shc, 2)):
        kb.op("dve", lambda e: e.tensor_scalar(out=g1[:], in0=mv[:, :, o + 1], scalar1=1.0, scalar2=None, op0=ALU.add),
              r=[tag + "mv"], w=[tag + "g"])
        kb.op("dve", lambda e: e.tensor_tensor(out=g1[:], in0=g1[:], in1=nw[:], op=ALU.mult), r=[tag + "g", tag + "nw"], w=[tag + "g"])
        kb.op("dve", lambda e: e.tensor_copy(out=sh[:], in_=mv[:, :, o]), r=[tag + "mv"], w=[tag + "g"])
    return g1l, shl, g1c, shc


def make_ones(kb, dt=BF16, name="ones"):
    o = kb.sb([128, 128], dt, name=name)
    kb.op("dve", lambda e: e.memset(o[:], 1.0), w=[name])
    return o


def build_proj():
    kb = KB()
    T = T_CORE
    xT = kb.dram("xT", [D, T])
    nw_d = kb.dram("nw", [128, 16])
    mv_d = kb.dram("mv", [128, 16, 4])
    w = kb.dram("w", [D, NW])
    out = kb.dram("out", [T, NW], kind="ExternalOutput")
    ones_b = make_ones(kb)
    g1l, shl, g1c, shc = emit_mod_vectors(kb, nw_d, mv_d, "m")
    hT = kb.sb([128, 16, T], BF16, name="hT")
    emit_norm_mod(kb, xT, hT, ones_b, g1l, shl, g1c, shc, "n")
    hkeys = [("nhT", gi) for gi in range(len(TGROUPS))]
    wv = w.rearrange("(kc p) n -> p kc n", p=128)
    wbs = [kb.sb([128, 16, 512], BF16, name=f"wb{i}") for i in range(2)]
    pss = [kb.ps([128, 512], name=f"pp{i}") for i in range(4)]
    sts = [kb.sb([128, 512], name=f"st{i}") for i in range(4)]
    ngrp = (NW + 511) // 512
    it = 0
    for g in range(ngrp):
        c0 = g * 512
        nco = min(512, NW - c0)
        wb = wbs[g % 2]
        kb.dma("pool", wb[:, :, 0:nco], wv[:, :, c0:c0 + nco], w=[("wb", g % 2)])
        for tl in range(T // 128):
            ps = pss[it % 4]
            st = sts[it % 4]
            for kc in range(16):
                kb.op("pe", lambda e: e.matmul(ps[:, 0:nco], lhsT=hT[:, kc, tl * 128:(tl + 1) * 128], rhs=wb[:, kc, 0:nco],
                                               start=(kc == 0), stop=(kc == 15)),
                      r=[("wb", g % 2)] + hkeys, w=[("pp", it % 4)])
            if it % 2 == 0:
                kb.op("act", lambda e: e.activation(out=st[:, 0:nco], in_=ps[:, 0:nco], func=AF.Copy), r=[("pp", it % 4)], w=[("st", it % 4)])
            else:
                kb.op("dve", lambda e: e.tensor_copy(out=st[:, 0:nco], in_=ps[:, 0:nco]), r=[("pp", it % 4)], w=[("st", it % 4)])
            kb.dma("sp", out[tl * 128:(tl + 1) * 128, c0:c0 + nco], st[:, 0:nco], r=[("st", it % 4)], w=["out"])
            it += 1
    kb.finish(["out"])
    return kb


def fm16(v):
    return np.ascontiguousarray(v.reshape(16, 128).T)


ROPE_PERM = np.concatenate([np.arange(16, 32), np.arange(0, 16), np.arange(48, 64), np.arange(32, 48)])


def tokens_of_core(xfull, xcfull, i):
    b, hf = i // 2, i % 2
    xs = np.concatenate([xfull[b, hf * 2048:(hf + 1) * 2048], xcfull[b, hf * 128:(hf + 1) * 128]], axis=0)
    return np.ascontiguousarray(xs.T)


def run_proj(x, xc, mod, l, inp, kb=None):
    w_in = inp["w_in"][l]
    kro = sum((1024, 1024, 1024, 1024, 1024, 1024, 512, 512))
    wext = np.concatenate([w_in, w_in[:, kro:kro + 64][:, ROPE_PERM]], axis=1)
    nw = fm16(inp["norm_mix"][l])
    kb = kb or build_proj()
    maps = []
    for i in range(NCORE):
        b = i // 2
        mv = np.stack([fm16(mod[b, l, 0]), fm16(mod[b, l, 1]), fm16(mod[4, l, 0]), fm16(mod[4, l, 1])], axis=-1)
        maps.append({"xT": tokens_of_core(x, xc, i), "nw": nw, "mv": np.ascontiguousarray(mv), "w": wext})
    res = run(kb, maps)
    P_lat = np.empty((B, SEQ, NW), np.float32)
    P_ctx = np.empty((B, CTX, NW), np.float32)
    for i in range(NCORE):
        b, hf = i // 2, i % 2
        o = res[i]["out"]
        P_lat[b, hf * 2048:(hf + 1) * 2048] = o[:2048]
        P_ctx[b, hf * 128:(hf + 1) * 128] = o[2048:]
    return P_lat, P_ctx


HL = LTOT
HC = 32
NREC = 8


def build_hgrn(nrec=NREC):
    kb = KB()
    L = HL
    BK = 64
    NB = L // BK
    qT_d = kb.dram("qT", [nrec, 128, L])
    fT_d = kb.dram("fT", [nrec, 128, L])
    v_d = kb.dram("v", [nrec, L, 128])
    lg_d = kb.dram("lg", [nrec, 128, 4])
    smask_d = kb.dram("smask", [128, L])
    m01_d = kb.dram("m01", [128, 128])
    id_d = kb.dram("ident", [128, 128])
    oT_d = kb.dram("oT", [nrec, 128, L], kind="ExternalOutput")

    smask = kb.sb([128, L], name="smask")
    m01 = kb.sb([128, 128], name="m01")
    ident = kb.sb([128, 128], BF16, name="ident")
    kb.dma("sp", smask[:], smask_d, w=["smask"])
    kb.dma("sp", m01[:], m01_d, w=["m01"])
    kb.dma("pool", ident[:], id_d, w=["ident"])

    qT = kb.sb([128, L], name="qT")
    fT = kb.sb([128, L], name="fT")
    vb = kb.sb([BK, NB, 128], BF16, name="vb")
    lg = kb.sb([128, 4], name="lg")
    lbv = kb.sb([128, 4], name="lbv")
    w1 = kb.sb([128, L], name="w1")
    w2 = kb.sb([128, L], name="w2")
    kf = kb.sb([128, L], name="kf")
    qt_b = kb.sb([128, L], BF16, name="qt_b")
    kt_b = kb.sb([128, L], BF16, name="kt_b")
    kh_b = kb.sb([128, L], BF16, name="kh_b")
    etot = kb.sb([128, L // HC], name="etot")
    khtok = kb.sb([128, 128], BF16, name="khtok")
    atm = kb.sb([128, 128], BF16, name="atm")
    S32 = kb.sb([128, 128], name="S32")
    S16 = kb.sb([128, 128], BF16, name="S16")
    oin = kb.sb([128, 128], name="oin")
    osb = [kb.sb([128, 128], name=f"osb{i}") for i in range(2)]
    ps_tr = kb.ps([128, 128], BF16, name="ps_tr")
    ps_at = kb.ps([128, 128], name="ps_at")
    ps_oi = kb.ps([128, 128], name="ps_oi")
    ps_ox = kb.ps([128, 128], name="ps_ox")
    ps_s = kb.ps([128, 128], name="ps_s")

    for r in range(nrec):
        kb.dma("sp", qT[:], qT_d[r], w=["qT"])
        kb.dma("sp", fT[:], fT_d[r], w=["fT"])
        kb.dma("sp", lg[:], lg_d[r], w=["lg"])
        kb.dma("pool", vb[:], v_d[r].rearrange("(n p) c -> p n c", p=BK), w=["vb"])
        kb.op("act", lambda e: e.activation(out=lbv[:, 0:2], in_=lg[:, 0:2], func=AF.Exp), r=["lg"], w=["lbv"])
        kb.op("dve", lambda e: e.tensor_tensor(out=lbv[:, 2:3], in0=lbv[:, 0:1], in1=lbv[:, 1:2], op=ALU.add), r=["lbv"], w=["lbv"])
        kb.op("dve", lambda e: e.reciprocal(out=lbv[:, 2:3], in_=lbv[:, 2:3]), r=["lbv"], w=["lbv"])
        kb.op("dve", lambda e: e.tensor_tensor(out=lbv[:, 0:1], in0=lbv[:, 1:2], in1=lbv[:, 2:3], op=ALU.mult), r=["lbv"], w=["lbv"])
        kb.op("dve", lambda e: e.tensor_tensor(out=lbv[:, 0:1], in0=lbv[:, 0:1], in1=lg[:, 2:3], op=ALU.mult), r=["lbv", "lg"], w=["lbv"])
        kb.op("dve", lambda e: e.tensor_scalar(out=lbv[:, 1:2], in0=lbv[:, 0:1], scalar1=-1.0, scalar2=1.0, op0=ALU.mult, op1=ALU.add),
              r=["lbv"], w=["lbv"])
        kb.op("act", lambda e: e.activation(out=w1[:], in_=fT[:], func=AF.Sigmoid), r=["fT"], w=["w1"])
        kb.op("dve", lambda e: e.tensor_scalar(out=w1[:], in0=w1[:], scalar1=lbv[:, 1:2], scalar2=lbv[:, 0:1], op0=ALU.mult, op1=ALU.add),
              r=["w1", "lbv"], w=["w1"])
        kb.op("dve", lambda e: e.tensor_scalar(out=kf[:], in0=w1[:], scalar1=-1.0, scalar2=1.0, op0=ALU.mult, op1=ALU.add), r=["w1"], w=["kf"])
        kb.op("act", lambda e: e.activation(out=w1[:], in_=w1[:], func=AF.Ln), r=["w1"], w=["w1"])
        kb.op("dve", lambda e: e.tensor_tensor_scan(out=w2[:], data0=smask[:], data1=w1[:], initial=0.0, op0=ALU.mult, op1=ALU.add),
              r=["w1", "smask"], w=["w2"])
        kb.op("act", lambda e: e.activation(out=w1[:], in_=w2[:], func=AF.Exp), r=["w2"], w=["w1"])
        kb.op("act", lambda e: e.activation(out=qT[:], in_=qT[:], func=AF.Silu), r=["qT"], w=["qT"])
        kb.op("dve", lambda e: e.tensor_tensor(out=qt_b[:], in0=qT[:], in1=w1[:], op=ALU.mult), r=["qT", "w1"], w=["qt_b"])
        kb.op("act", lambda e: e.activation(out=w1[:], in_=w2[:], func=AF.Exp, scale=-1.0), r=["w2"], w=["w1"])
        kb.op("dve", lambda e: e.tensor_tensor(out=kt_b[:], in0=kf[:], in1=w1[:], op=ALU.mult), r=["kf", "w1"], w=["kt_b"])
        b3 = w2[:].rearrange("p (n c) -> p n c", c=HC)
        kb.op("act", lambda e: e.activation(out=etot[:], in_=b3[:, :, HC - 1], func=AF.Exp), r=["w2"], w=["etot"])
        kb.op("dve", lambda e: e.tensor_tensor(out=w1[:].rearrange("p (n c) -> p n c", c=HC), in0=b3[:, :, HC - 1:HC].broadcast_to([128, L // HC, HC]),
                                               in1=b3, op=ALU.subtract), r=["w2"], w=["w1"])
        kb.op("act", lambda e: e.activation(out=w1[:], in_=w1[:], func=AF.Exp), r=["w1"], w=["w1"])
        kb.op("dve", lambda e: e.tensor_tensor(out=kh_b[:], in0=kf[:], in1=w1[:], op=ALU.mult), r=["kf", "w1"], w=["kh_b"])
        kb.op("dve", lambda e: e.memset(S32[:], 0.0), w=["S32"])
        kb.op("dve", lambda e: e.memset(S16[:], 0.0), w=["S16"])
        for j in range(NB):
            sl = slice(j * BK, (j + 1) * BK)
            kb.op("pe", lambda e: e.transpose(ps_tr[0:BK, :], kh_b[:, sl], ident[:]), r=["kh_b", "ident"], w=["ps_tr"])
            kb.op("act", lambda e: e.activation(out=khtok[0:BK, :], in_=ps_tr[0:BK, :], func=AF.Copy), r=["ps_tr"], w=["khtok"])
            kb.op("pe", lambda e: e.matmul(ps_at[0:BK, 0:BK], lhsT=kt_b[:, sl], rhs=qt_b[:, sl], start=True, stop=True), r=["kt_b", "qt_b"], w=["ps_at"])
            kb.op("dve", lambda e: e.tensor_tensor(out=atm[0:BK, 0:BK], in0=ps_at[0:BK, 0:BK], in1=m01[0:BK, 0:BK], op=ALU.mult), r=["ps_at", "m01"], w=["atm"])
            kb.op("pe", lambda e: e.matmul(ps_oi[:, 0:BK], lhsT=vb[:, j, :], rhs=atm[0:BK, 0:BK], start=True, stop=True), r=["vb", "atm"], w=["ps_oi"])
            for i in range(BK // HC):
                c0 = j * BK + i * HC
                pr = slice(i * HC, (i + 1) * HC)
                kb.op("pe", lambda e: e.matmul(ps_ox[:, pr], lhsT=S16[:], rhs=qt_b[:, c0:c0 + HC], start=True, stop=True),
                      r=["S16", "qt_b"], w=[("ps_ox", i)])
                kb.op("pe", lambda e: e.matmul(ps_s[:], lhsT=khtok[pr, :], rhs=vb[pr, j, :], start=True, stop=True),
                      r=["khtok", "vb"], w=["ps_s"])
                ci = j * (BK // HC) + i
                kb.op("dve", lambda e: e.scalar_tensor_tensor(out=S32[:], in0=S32[:], scalar=etot[:, ci:ci + 1], in1=ps_s[:], op0=ALU.mult, op1=ALU.add),
                      r=["S32", "etot", "ps_s"], w=["S32"])
                kb.op("act", lambda e: e.activation(out=S16[:], in_=S32[:], func=AF.Copy), r=["S32"], w=["S16"])
            kb.op("act", lambda e: e.activation(out=oin[:, 0:BK], in_=ps_ox[:, 0:BK], func=AF.Copy), r=[("ps_ox", i) for i in range(BK // HC)], w=["oin"])
            ob = osb[j % 2]
            kb.op("dve", lambda e: e.tensor_tensor(out=ob[:, 0:BK], in0=ps_oi[:, 0:BK], in1=oin[:, 0:BK], op=ALU.add), r=["ps_oi", "oin"], w=[("osb", j % 2)])
            kb.dma("sp", oT_d[r, :, sl], ob[:, 0:BK], r=[("osb", j % 2)], w=["oT"])
    kb.finish(["oT"])
    return kb


def hgrn_consts():
    sm = np.ones((128, HL), np.float32)
    sm[:, ::HC] = 0.0
    s = np.arange(128)[:, None]
    t = np.arange(128)[None, :]
    m01 = ((s // HC == t // HC) & (s <= t)).astype(np.float32)
    return sm, m01, np.eye(128, dtype=np.float32)


MH = 8
MLA_SCALE = (128 + 64) ** -0.5


def build_mla():
    kb = KB()
    L = LTOT
    NT = L // 128
    cq_d = kb.dram("cqT", [512, L])
    ckv_d = kb.dram("ckvT", [512, L])
    kr_d = kb.dram("krT", [64, L])
    krs_d = kb.dram("krsT", [64, L])
    cos_d = kb.dram("cosT", [64, L])
    sin_d = kb.dram("sinT", [64, L])
    nq_d = kb.dram("nq", [128, 4])
    nkv_d = kb.dram("nkv", [128, 4])
    wqn_d = kb.dram("wqn", [512, MH * 128])
    wqr_d = kb.dram("wqr", [512, MH * 64])
    wqs_d = kb.dram("wqs", [512, MH * 64])
    wkn_d = kb.dram("wkn", [512, MH * 128])
    wvv_d = kb.dram("wvv", [512, MH * 128])
    att_d = kb.dram("att", [L, MH * 128], kind="ExternalOutput")

    ones_b = make_ones(kb)
    cosT = kb.sb([64, L], name="cosT")
    sinT = kb.sb([64, L], name="sinT")
    nq = kb.sb([128, 4], name="nq")
    nkv = kb.sb([128, 4], name="nkv")
    kb.dma("sp", cosT[:], cos_d, w=["cosT"])
    kb.dma("sp", sinT[:], sin_d, w=["sinT"])
    kb.dma("sp", nq[:], nq_d, w=["nq"])
    kb.dma("sp", nkv[:], nkv_d, w=["nkv"])
    wts = {}
    for nm, dd, n in (("wqn", wqn_d, MH * 128), ("wqr", wqr_d, MH * 64), ("wqs", wqs_d, MH * 64), ("wkn", wkn_d, MH * 128), ("wvv", wvv_d, MH * 128)):
        t = kb.sb([128, 4, n], BF16, name=nm)
        kb.dma("pool", t[:], dd.rearrange("(kc p) n -> p kc n", p=128), w=[nm])
        wts[nm] = t
    cqn = kb.sb([128, 4, L], BF16, name="cqn")
    ckvn = kb.sb([128, 4, L], BF16, name="ckvn")
    KrT = kb.sb([64, L], BF16, name="KrT")
    xg = kb.sb([128, 4, 512], name="xg")
    sqg = kb.sb([128, 4, 512], BF16, name="sqg")
    rsg = kb.sb([128, 512], name="rsg")
    krg = kb.sb([64, 2, 512], name="krg")
    ps_a = kb.ps([128, 512], name="ps_a")
    groups = [(g * 512, min(512, L - g * 512)) for g in range((L + 511) // 512)]
    for (t0, n) in groups:
        for (src_d, nrm, dst, nm) in ((cq_d, nq, cqn, "cqn"), (ckv_d, nkv, ckvn, "ckvn")):
            kb.dma("sp", xg[:, :, 0:n], src_d.rearrange("(kc p) t -> p kc t", p=128)[:, :, t0:t0 + n], w=["xg"])
            kb.op("act", lambda e: e.activation(out=sqg[:, :, 0:n], in_=xg[:, :, 0:n], func=AF.Square), r=["xg"], w=["sqg"])
            for kc in range(4):
                kb.op("pe", lambda e: e.matmul(ps_a[:, 0:n], lhsT=ones_b[:], rhs=sqg[:, kc, 0:n], start=(kc == 0), stop=(kc == 3)),
                      r=["sqg", "ones"], w=["ps_a"])
            kb.op("dve", lambda e: e.tensor_scalar(out=rsg[:, 0:n], in0=ps_a[:, 0:n], scalar1=1.0 / 512, scalar2=EPS, op0=ALU.mult, op1=ALU.add),
                  r=["ps_a"], w=["rsg"])
            kb.op("act", lambda e: e.activation(out=rsg[:, 0:n], in_=rsg[:, 0:n], func=AF.Sqrt), r=["rsg"], w=["rsg"])
            kb.op("dve", lambda e: e.reciprocal(out=rsg[:, 0:n], in_=rsg[:, 0:n]), r=["rsg"], w=["rsg"])
            for kc in range(4):
                kb.op("dve", lambda e: e.scalar_tensor_tensor(out=dst[:, kc, t0:t0 + n], in0=xg[:, kc, 0:n], scalar=nrm[:, kc:kc + 1], in1=rsg[:, 0:n],
                                                              op0=ALU.mult, op1=ALU.mult), r=["xg", "rsg", "nq", "nkv"], w=[nm])
        kb.dma("sp", krg[:, 0, 0:n], kr_d[:, t0:t0 + n], w=["krg"])
        kb.dma("sp", krg[:, 1, 0:n], krs_d[:, t0:t0 + n], w=["krg"])
        kb.op("dve", lambda e: e.tensor_tensor(out=krg[:, 0, 0:n], in0=krg[:, 0, 0:n], in1=cosT[:, t0:t0 + n], op=ALU.mult), r=["krg", "cosT"], w=["krg"])
        kb.op("dve", lambda e: e.tensor_tensor(out=krg[:, 1, 0:n], in0=krg[:, 1, 0:n], in1=sinT[:, t0:t0 + n], op=ALU.mult), r=["krg", "sinT"], w=["krg"])
        kb.op("dve", lambda e: e.tensor_tensor(out=KrT[:, t0:t0 + n], in0=krg[:, 0, 0:n], in1=krg[:, 1, 0:n], op=ALU.add), r=["krg"], w=["KrT"])
    QnT = kb.sb([128, L], BF16, name="QnT")
    QrT = kb.sb([64, L], BF16, name="QrT")
    KnT = kb.sb([128, L], BF16, name="KnT")
    Vh = kb.sb([128, NT, 129], BF16, name="Vh")
    qtmp = kb.sb([64, 512], name="qtmp")
    pT = [kb.sb([128, 512], BF16, name=f"pT{i}") for i in range(2)]
    ps_s = [kb.ps([128, 512], name=f"ps_s{i}") for i in range(2)]
    ps_o = [kb.ps([128, 512], name=f"ps_o{i}") for i in range(4)]
    ps_b = kb.ps([128, 512], name="ps_b")
    rec = kb.sb([128, 4], name="rec")
    ost = [kb.sb([128, 128], name=f"ost{i}") for i in range(2)]
    kb.op("dve", lambda e: e.memset(Vh[:, :, 128:129], 1.0), w=["Vh1"])
    nst = 0
    for h in range(MH):
        for (t0, n) in groups:
            for (wn, src, dst, nm, rows, c0, cw) in (("wqn", cqn, QnT, "QnT", 128, h * 128, 128), ("wkn", ckvn, KnT, "KnT", 128, h * 128, 128)):
                for kc in range(4):
                    kb.op("pe", lambda e: e.matmul(ps_a[0:rows, 0:n], lhsT=wts[wn][:, kc, c0:c0 + cw], rhs=src[:, kc, t0:t0 + n], start=(kc == 0), stop=(kc == 3)),
                          r=[wn, "cqn", "ckvn"], w=["ps_a"])
                kb.op("act", lambda e: e.activation(out=dst[:, t0:t0 + n], in_=ps_a[0:rows, 0:n], func=AF.Copy), r=["ps_a"], w=[nm])
            for kc in range(4):
                kb.op("pe", lambda e: e.matmul(ps_a[0:64, 0:n], lhsT=wts["wqr"][:, kc, h * 64:(h + 1) * 64], rhs=cqn[:, kc, t0:t0 + n], start=(kc == 0), stop=(kc == 3)),
                      r=["wqr", "cqn"], w=["ps_a"])
            for kc in range(4):
                kb.op("pe", lambda e: e.matmul(ps_b[0:64, 0:n], lhsT=wts["wqs"][:, kc, h * 64:(h + 1) * 64], rhs=cqn[:, kc, t0:t0 + n], start=(kc == 0), stop=(kc == 3)),
                      r=["wqs", "cqn"], w=["ps_b"])
            kb.op("dve", lambda e: e.tensor_tensor(out=qtmp[:, 0:n], in0=ps_a[0:64, 0:n], in1=cosT[:, t0:t0 + n], op=ALU.mult), r=["ps_a", "cosT"], w=["qtmp"])
            kb.op("dve", lambda e: e.tensor_tensor(out=QrT[:, t0:t0 + n], in0=ps_b[0:64, 0:n], in1=sinT[:, t0:t0 + n], op=ALU.mult), r=["ps_b", "sinT"], w=["QrT"])
            kb.op("dve", lambda e: e.tensor_tensor(out=QrT[:, t0:t0 + n], in0=QrT[:, t0:t0 + n], in1=qtmp[:, 0:n], op=ALU.add), r=["QrT", "qtmp"], w=["QrT"])
        for kt in range(NT):
            for kc in range(4):
                kb.op("pe", lambda e: e.matmul(ps_a[:, 0:128], lhsT=ckvn[:, kc, kt * 128:(kt + 1) * 128], rhs=wts["wvv"][:, kc, h * 128:(h + 1) * 128], start=(kc == 0), stop=(kc == 3)),
                      r=["wvv", "ckvn"], w=["ps_a"])
            kb.op("act", lambda e: e.activation(out=Vh[:, kt, 0:128], in_=ps_a[:, 0:128], func=AF.Copy), r=["ps_a"], w=["Vh"])
        qgroups = [(0, 256, 2)] + [(256 + g * 512, 512, NT) for g in range(8)]
        for (q0, qn, nkt) in qgroups:
            nqs = qn // 128
            for kt in range(nkt):
                pss = ps_s[kt % 2]
                pt = pT[kt % 2]
                kb.op("pe", lambda e: e.matmul(pss[:, 0:qn], lhsT=KnT[:, kt * 128:(kt + 1) * 128], rhs=QnT[:, q0:q0 + qn], start=True, stop=False),
                      r=["KnT", "QnT"], w=[("ps_s", kt % 2)])
                kb.op("pe", lambda e: e.matmul(pss[:, 0:qn], lhsT=KrT[:, kt * 128:(kt + 1) * 128], rhs=QrT[:, q0:q0 + qn], start=False, stop=True),
                      r=["KrT", "QrT"], w=[("ps_s", kt % 2)])
                kb.op("act", lambda e: e.activation(out=pt[:, 0:qn], in_=pss[:, 0:qn], func=AF.Exp, scale=MLA_SCALE), r=[("ps_s", kt % 2)], w=[("pT", kt % 2)])
                for qs in range(nqs):
                    kb.op("pe", lambda e: e.matmul(ps_o[qs][:, 0:129], lhsT=pt[:, qs * 128:(qs + 1) * 128], rhs=Vh[:, kt, :], start=(kt == 0), stop=(kt == nkt - 1)),
                          r=[("pT", kt % 2), "Vh", "Vh1"], w=[("ps_o", qs)])
            for qs in range(nqs):
                kb.op("dve", lambda e: e.reciprocal(out=rec[:, qs:qs + 1], in_=ps_o[qs][:, 128:129]), r=[("ps_o", qs)], w=[("rec", qs)])
                o = ost[nst % 2]
                kb.op("act", lambda e: e.activation(out=o[:], in_=ps_o[qs][:, 0:128], func=AF.Copy, scale=rec[:, qs:qs + 1]), r=[("ps_o", qs), ("rec", qs)], w=[("ost", nst % 2)])
                kb.dma("sp", att_d[q0 + qs * 128:q0 + (qs + 1) * 128, h * 128:(h + 1) * 128], o[:], r=[("ost", nst % 2)], w=["att"])
                nst += 1
    kb.finish(["att"])
    return kb


def rope_tables():
    pos = np.arange(SEQ)
    r, col = pos // 64, pos % 64
    freqs = (10000.0 ** (-np.arange(16, dtype=np.float32) / 16)).astype(np.float32)
    ang = np.stack([r[:, None].astype(np.float32) * freqs, col[:, None].astype(np.float32) * freqs], axis=1)
    cos, sin = np.cos(ang).astype(np.float32), np.sin(ang).astype(np.float32)
    cosT = np.ones((64, LTOT), np.float32)
    sinT = np.zeros((64, LTOT), np.float32)
    for a in range(2):
        for j in range(2):
            rows = slice(a * 32 + j * 16, a * 32 + j * 16 + 16)
            cosT[rows, CTX:] = cos[:, a, :].T
            sinT[rows, CTX:] = (-sin[:, a, :].T) if j == 0 else sin[:, a, :].T
    return cosT, sinT


PH = 8
UW = (2048 + 2 * PH) + (128 + 2 * PH)


def build_merge():
    kb = KB()
    T = T_CORE
    xT_d = kb.dram("xT", [D, T])
    oF_d = kb.dram("oF", [1024, T])
    oB_d = kb.dram("oB", [1024, T])
    g_d = kb.dram("gT", [1024, T])
    u_d = kb.dram("uT", [1024, UW])
    att_d = kb.dram("attT", [D, T])
    gr_d = kb.dram("grT", [3 * D, T])
    hn_d = kb.dram("hn", [128, 1])
    psc_d = kb.dram("psc", [128, 8])
    icnt_d = kb.dram("icnt", [128, 4, T])
    pw_d = kb.dram("pw", [4, 256, 256])
    wa_d = kb.dram("wa", [1024, D])
    wb_d = kb.dram("wb", [1024, D])
    wc_d = kb.dram("wc", [D, D])
    wo_d = kb.dram("wo", [D, D])
    gm_d = kb.dram("gm", [128, 16, 2])
    out_d = kb.dram("x1T", [D, T], kind="ExternalOutput")

    ones_b = make_ones(kb)
    hn = kb.sb([128, 1], name="hn")
    psc = kb.sb([128, 8], name="psc")
    gm = kb.sb([128, 16, 2], name="gm")
    pw = kb.sb([128, 4, 2, 256], BF16, name="pw")
    kb.dma("sp", hn[:], hn_d, w=["hn"])
    kb.dma("sp", psc[:], psc_d, w=["psc"])
    kb.dma("sp", gm[:], gm_d, w=["gm"])
    kb.dma("pool", pw[:], pw_d.rearrange("g (kc p) o -> p g kc o", p=128), w=["pw"])

    yT = kb.sb([128, 32, 512], BF16, name="yT")
    mT = kb.sb([128, 16, 512], BF16, name="mT")
    a1 = kb.sb([128, 512], name="a1")
    a2 = kb.sb([128, 512], name="a2")
    a3 = kb.sb([128, 512], name="a3")
    sqb = kb.sb([128, 512], BF16, name="sqb")
    uh = kb.sb([128, 512 + 2 * PH], name="uh")
    s2 = kb.sb([128, 512 + 2 * PH], name="s2")
    s3 = kb.sb([128, 512 + 2 * PH], name="s3")
    icn = kb.sb([128, 512], name="icn")
    plb = kb.sb([128, 2, 512], BF16, name="plb")
    wbr = [kb.sb([128, 16, 512], BF16, name=f"wbr{i}") for i in range(2)]
    gsb = kb.sb([128, 3, 512], name="gsb")
    xg = kb.sb([128, 512], name="xg")
    og = [kb.sb([128, 512], name=f"og{i}") for i in range(2)]
    ps1 = kb.ps([128, 512], name="ps1")
    psb = [kb.ps([128, 512], name=f"psb{i}") for i in range(3)]
    pso = [kb.ps([128, 512], name=f"pso{i}") for i in range(2)]
    nw = 0
    no = 0
    for (t0, n) in TGROUPS:
        isctx = t0 >= 2048
        for hd in range(8):
            rows = slice(hd * 128, (hd + 1) * 128)
            kb.dma("sp", a1[:, 0:n], oF_d[rows, t0:t0 + n], w=["a1"])
            kb.dma("sp", a2[:, 0:n], oB_d[rows, t0:t0 + n], w=["a2"])
            kb.dma("sp", a3[:, 0:n], g_d[rows, t0:t0 + n], w=["a3"])
            kb.op("dve", lambda e: e.tensor_tensor(out=a1[:, 0:n], in0=a1[:, 0:n], in1=a2[:, 0:n], op=ALU.add), r=["a1", "a2"], w=["a1"])
            kb.op("act", lambda e: e.activation(out=sqb[:, 0:n], in_=a1[:, 0:n], func=AF.Square), r=["a1"], w=["sqb"])
            kb.op("pe", lambda e: e.matmul(ps1[:, 0:n], lhsT=ones_b[:], rhs=sqb[:, 0:n], start=True, stop=True), r=["sqb", "ones"], w=["ps1"])
            kb.op("dve", lambda e: e.tensor_scalar(out=a2[:, 0:n], in0=ps1[:, 0:n], scalar1=1.0 / 128, scalar2=EPS, op0=ALU.mult, op1=ALU.add), r=["ps1"], w=["a2"])
            kb.op("act", lambda e: e.activation(out=a2[:, 0:n], in_=a2[:, 0:n], func=AF.Sqrt), r=["a2"], w=["a2"])
            kb.op("dve", lambda e: e.reciprocal(out=a2[:, 0:n], in_=a2[:, 0:n]), r=["a2"], w=["a2"])
            kb.op("act", lambda e: e.activation(out=a3[:, 0:n], in_=a3[:, 0:n], func=AF.Silu), r=["a3"], w=["a3"])
            kb.op("dve", lambda e: e.scalar_tensor_tensor(out=a1[:, 0:n], in0=a1[:, 0:n], scalar=hn[:, 0:1], in1=a2[:, 0:n], op0=ALU.mult, op1=ALU.mult),
                  r=["a1", "a2", "hn"], w=["a1"])
            kb.op("dve", lambda e: e.tensor_tensor(out=yT[:, hd, 0:n], in0=a1[:, 0:n], in1=a3[:, 0:n], op=ALU.mult), r=["a1", "a3"], w=[("yT", hd)])
        u0 = (t0) if not isctx else (2048 + 2 * PH + (t0 - 2048))
        for gi, win in enumerate((2, 4, 8, 16)):
            kb.dma("sp", icn[:, 0:n], icnt_d[:, gi, t0:t0 + n], w=["icn"])
            for cc in range(2):
                ch = gi * 2 + cc
                kb.dma("sp", uh[:, 0:n + 2 * PH], u_d[ch * 128:(ch + 1) * 128, u0:u0 + n + 2 * PH], w=["uh"])
                W_ = n + 2 * PH
                kb.op("dve", lambda e: e.tensor_tensor(out=s2[:, 0:W_ - 1], in0=uh[:, 0:W_ - 1], in1=uh[:, 1:W_], op=ALU.add), r=["uh"], w=["s2"])
                step = 2
                while step < win:
                    kb.op("dve", lambda e: e.tensor_tensor(out=s3[:, 0:0 + W_ - 2 * step + 1], in0=s2[:, 0:W_ - 2 * step + 1], in1=s2[:, step:W_ - step + 1], op=ALU.add),
                          r=["s2"], w=["s3"])
                    kb.op("dve", lambda e: e.tensor_copy(out=s2[:, 0:W_ - 2 * step + 1], in_=s3[:, 0:W_ - 2 * step + 1]), r=["s3"], w=["s2"])
                    step *= 2
                st = PH - win // 2
                kb.op("dve", lambda e: e.tensor_tensor(out=a1[:, 0:n], in0=s2[:, st:st + n], in1=icn[:, 0:n], op=ALU.mult), r=["s2", "icn"], w=["a1"])
                kb.op("dve", lambda e: e.tensor_tensor(out=plb[:, cc, 0:n], in0=a1[:, 0:n], in1=uh[:, PH:PH + n], op=ALU.subtract), r=["a1", "uh"], w=["plb"])
            for oc in range(2):
                for kc in range(2):
                    kb.op("pe", lambda e: e.matmul(ps1[:, 0:n], lhsT=pw[:, gi, kc, oc * 128:(oc + 1) * 128], rhs=plb[:, kc, 0:n], start=(kc == 0), stop=(kc == 1)),
                          r=["pw", "plb"], w=["ps1"])
                ch = gi * 2 + oc
                kb.op("act", lambda e: e.activation(out=yT[:, 8 + ch, 0:n], in_=ps1[:, 0:n], func=AF.Copy, scale=psc[:, ch:ch + 1]), r=["ps1", "psc"], w=[("yT", 8 + ch)])
        kb.dma("pool", yT[:, 16:32, 0:n], att_d.rearrange("(kc p) t -> p kc t", p=128)[:, :, t0:t0 + n], w=[("yT", 16 + i) for i in range(16)])
        ykeys = [("yT", i) for i in range(32)]
        for cb in range(4):
            brs = (("a", wa_d, 0, 8), ("b", wb_d, 8, 8), ("c", wc_d, 16, 16))
            wtl = []
            for (bn, wd, y0, nk) in brs:
                wt = wbr[nw % 2]
                kb.dma("pool", wt[:, 0:nk, :], wd.rearrange("(kc p) n -> p kc n", p=128)[:, :, cb * 512:(cb + 1) * 512], w=[("wbr", nw % 2)])
                for dc in range(4):
                    pass
                wtl.append((wt, ("wbr", nw % 2), y0, nk))
                nw += 1
                bi = len(wtl) - 1
                for dc in range(4):
                    dcg = cb * 4 + dc
                    if bi == 0:
                        kb.dma("sp", gsb[:, :, 0:n], gr_d.rearrange("(b kc p) t -> p b kc t", p=128, b=3)[:, :, dcg, t0:t0 + n], w=[("gsb", dc)]) if False else None
                    ps = psb[bi]
                    for kc in range(nk):
                        kb.op("pe", lambda e: e.matmul(ps[:, 0:n], lhsT=wt[:, kc, dc * 128:(dc + 1) * 128], rhs=yT[:, y0 + kc, 0:n], start=(kc == 0), stop=(kc == nk - 1)),
                              r=[("wbr", (nw - 1) % 2)] + ykeys, w=[("psb", bi)])
                    kb.dma("sp", a2[:, 0:n], gr_d[bi * D + dcg * 128: bi * D + (dcg + 1) * 128, t0:t0 + n], w=["a2"])
                    kb.op("act", lambda e: e.activation(out=a2[:, 0:n], in_=a2[:, 0:n], func=AF.Sigmoid), r=["a2"], w=["a2"])
                    if bi == 0:
                        kb.op("dve", lambda e: e.tensor_tensor(out=gsb[:, 0, 0:n] if False else mT32(kb, dc)[:, 0:n], in0=ps[:, 0:n], in1=a2[:, 0:n], op=ALU.mult),
                              r=[("psb", bi), "a2"], w=[("m32", dc)])
                    else:
                        kb.op("dve", lambda e: e.tensor_tensor(out=a1[:, 0:n], in0=ps[:, 0:n], in1=a2[:, 0:n], op=ALU.mult), r=[("psb", bi), "a2"], w=["a1"])
                        kb.op("dve", lambda e: e.tensor_tensor(out=mT32(kb, dc)[:, 0:n], in0=mT32(kb, dc)[:, 0:n], in1=a1[:, 0:n], op=ALU.add),
                              r=[("m32", dc), "a1"], w=[("m32", dc)])
                    if bi == 2:
                        kb.op("act", lambda e: e.activation(out=mT[:, dcg, 0:n], in_=mT32(kb, dc)[:, 0:n], func=AF.Copy), r=[("m32", dc)], w=[("mT", dcg)])
        mkeys = [("mT", i) for i in range(16)]
        for cb in range(4):
            wt = wbr[nw % 2]
            kb.dma("pool", wt[:, 0:16, :], wo_d.rearrange("(kc p) n -> p kc n", p=128)[:, :, cb * 512:(cb + 1) * 512], w=[("wbr", nw % 2)])
            wk = ("wbr", nw % 2)
            nw += 1
            for dc in range(4):
                dcg = cb * 4 + dc
                ps = pso[no % 2]
                for kc in range(16):
                    kb.op("pe", lambda e: e.matmul(ps[:, 0:n], lhsT=wt[:, kc, dc * 128:(dc + 1) * 128], rhs=mT[:, kc, 0:n], start=(kc == 0), stop=(kc == 15)),
                          r=[wk] + mkeys, w=[("pso", no % 2)])
                kb.dma("sp", xg[:, 0:n], xT_d[dcg * 128:(dcg + 1) * 128, t0:t0 + n], w=["xg"])
                o = og[no % 2]
                kb.op("dve", lambda e: e.scalar_tensor_tensor(out=o[:, 0:n], in0=ps[:, 0:n], scalar=gm[:, dcg, (1 if isctx else 0):(2 if isctx else 1)], in1=xg[:, 0:n],
                                                              op0=ALU.mult, op1=ALU.add), r=[("pso", no % 2), "xg", "gm"], w=[("og", no % 2)])
                kb.dma("sp", out_d[dcg * 128:(dcg + 1) * 128, t0:t0 + n], o[:, 0:n], r=[("og", no % 2)], w=["x1T"])
                no += 1
    kb.finish(["x1T"])
    return kb


_M32 = {}


def mT32(kb, dc):
    key = (id(kb), dc)
    if key not in _M32:
        _M32[key] = kb.sb([128, 512], name=f"m32_{dc}")
    return _M32[key]


NEG = -1.0e30


def build_peer():
    kb = KB()
    T = T_CORE
    NTL = T // 128
    xT_d = kb.dram("xT", [D, T])
    xtok_d = kb.dram("xtok", [T, D])
    nw_d = kb.dram("nw", [128, 16])
    mv_d = kb.dram("mv", [128, 16, 4])
    wq_d = kb.dram("wq", [D, D])
    keys_d = kb.dram("keysT", [16, 128, 128])
    uT_d = kb.dram("uT", [D, 16384])
    v_d = kb.dram("vtab", [16384, D])
    gf_d = kb.dram("gf", [2, 128, D])
    fn_d = kb.dram("fnw", [128, D])
    flag_d = kb.dram("flag", [128, 1])
    out_d = kb.dram("x2", [T, D], kind="ExternalOutput")

    ones_b = make_ones(kb)
    g1l, shl, g1c, shc = emit_mod_vectors(kb, nw_d, mv_d, "m")
    hT = kb.sb([128, 16, 128], BF16, name="hT")
    xtile = kb.sb([128, 16, 128], name="xtile")
    sq16 = kb.sb([128, 16, 128], BF16, name="sq16")
    rs = kb.sb([128, 128], name="rs")
    xv = xT_d.rearrange("(kc p) t -> p kc t", p=128)
    hkeys = ["hT"]
    ident = kb.sb([128, 128], BF16, name="ident")
    identf = kb.sb([128, 128], name="identf")
    kb.op("dve", lambda e: e.memset(identf[:], 0.0), w=["identf"])
    kb.op("pool", lambda e: e.affine_select(out=identf[:], in_=ones_f(kb)[:], pattern=[[-1, 128]], compare_op=ALU.is_equal, fill=0.0, base=0, channel_multiplier=1),
          r=["onesf"], w=["identf"])
    kb.op("dve", lambda e: e.tensor_copy(out=ident[:], in_=identf[:]), r=["identf"], w=["ident"])
    wqb = [kb.sb([128, 16, 128], BF16, name=f"wqb{i}") for i in range(2)]
    wqv = wq_d.rearrange("(kc p) n -> p kc n", p=128)
    keysT = kb.sb([128, 16, 128], BF16, name="keysT")
    kb.dma("pool", keysT[:], keys_d.rearrange("c d k -> d c k"), w=["keysT"])
    flag = kb.sb([128, 1], name="flag")
    kb.dma("sp", flag[:], flag_d, w=["flag"])

    qhT = kb.sb([128, 16, 128], BF16, name="qhT")
    sc = kb.sb([128, 16, 128], name="sc")
    tmp128 = kb.sb([128, 128], name="tmp128")
    top = kb.sb([128, 16, 16], name="top")
    cand = kb.sb([128, 256], name="cand")
    cand2 = kb.sb([128, 256], name="cand2")
    c16 = kb.sb([128, 16], name="c16")
    e16 = kb.sb([128, 16], name="e16")
    sm = kb.sb([128, 8, 8], name="sm")
    a1 = kb.sb([128, 8, 128], name="a1")
    P1 = kb.sb([128, 8, 128], name="P1")
    P2 = kb.sb([128, 8, 128], name="P2")
    sumb = kb.sb([128, 32, 128], name="sumb")
    ppb = kb.sb([128, 32, 128], name="ppb")
    wacc = kb.sb([128, 32, 128], name="wacc")
    ub = [kb.sb([128, 16, 512], BF16, name=f"ub{i}") for i in range(1)]
    vbuf = [kb.sb([128, 4, 512], BF16, name=f"vbuf{i}") for i in range(2)]
    ga = kb.sb([128, 512], name="ga")
    wtok = kb.sb([128, 512], BF16, name="wtok")
    wT = kb.sb([128, 4, 128], BF16, name="wT")
    xt = kb.sb([128, D], name="xt")
    gfr = kb.sb([128, D], name="gfr")
    fnr = kb.sb([128, D], name="fnr")
    sq = sumb[:, 0:16, :].rearrange("p a b -> p (a b)")
    st8 = kb.sb([128, 8], name="st8")
    ps_o = [kb.ps([128, 512], name=f"ps_o{i}") for i in range(4)]
    ps_act = [kb.ps([128, 512], name=f"ps_act{i}") for i in range(2)]
    ps_tr = kb.ps([128, 4, 128], BF16, name="ps_tr")
    ps_m = kb.ps([128, 512], name="ps_m")
    kb.dma("sp", fnr[:], fn_d, w=["fnr"])
    uv = uT_d.rearrange("(kc p) e -> p kc e", p=128)
    nu = 0
    nv = 0
    for tl in range(NTL):
        tsl = slice(tl * 128, (tl + 1) * 128)
        isctx = tl == NTL - 1
        kb.dma("sp", xtile[:], xv[:, :, tsl], w=["xtile"])
        kb.op("act", lambda e: e.activation(out=sq16[:], in_=xtile[:], func=AF.Square), r=["xtile"], w=["sq16"])
        for kc in range(16):
            kb.op("pe", lambda e: e.matmul(ps_m[:, 0:128], lhsT=ones_b[:], rhs=sq16[:, kc, :], start=(kc == 0), stop=(kc == 15)), r=["sq16", "ones"], w=["ps_m"])
        kb.op("dve", lambda e: e.tensor_scalar(out=rs[:], in0=ps_m[:, 0:128], scalar1=1.0 / D, scalar2=EPS, op0=ALU.mult, op1=ALU.add), r=["ps_m"], w=["rs"])
        kb.op("act", lambda e: e.activation(out=rs[:], in_=rs[:], func=AF.Sqrt), r=["rs"], w=["rs"])
        kb.op("dve", lambda e: e.reciprocal(out=rs[:], in_=rs[:]), r=["rs"], w=["rs"])
        kb.op("dve", lambda e: e.tensor_tensor(out=xtile[:], in0=xtile[:], in1=rs[:].unsqueeze(1).broadcast_to([128, 16, 128]), op=ALU.mult), r=["xtile", "rs"], w=["xtile"])
        g1_, sh_ = (g1c, shc) if isctx else (g1l, shl)
        for kc in range(16):
            kb.op("act", lambda e: e.activation(out=hT[:, kc, :], in_=xtile[:, kc, :], func=AF.Identity, scale=g1_[:, kc:kc + 1], bias=sh_[:, kc:kc + 1]),
                  r=["xtile", "mg"], w=["hT"])
        for c in range(16):
            wq_ = wqb[c % 2]
            kb.dma("pool", wq_[:], wqv[:, :, c * 128:(c + 1) * 128], w=[("wqb", c % 2)])
            for kc in range(16):
                kb.op("pe", lambda e: e.matmul(ps_m[:, 0:128], lhsT=wq_[:, kc, :], rhs=hT[:, kc, :], start=(kc == 0), stop=(kc == 15)),
                      r=[("wqb", c % 2)] + hkeys, w=["ps_m"])
            kb.op("act", lambda e: e.activation(out=qhT[:, c, :], in_=ps_m[:, 0:128], func=AF.Copy), r=["ps_m"], w=[("qhT", c)])
        for c in range(16):
            kb.op("pe", lambda e: e.matmul(ps_m[:, 0:128], lhsT=qhT[:, c, :], rhs=keysT[:, c, :], start=True, stop=True), r=[("qhT", c), "keysT"], w=["ps_m"])
            kb.op("act", lambda e: e.activation(out=sc[:, c, :], in_=ps_m[:, 0:128], func=AF.Copy), r=["ps_m"], w=[("sc", c)])
            kb.op("dve", lambda e: e.max(out=top[:, c, 0:8], in_=sc[:, c, :]), r=[("sc", c)], w=[("top", c)])
            kb.op("dve", lambda e: e.match_replace(out=tmp128[:], in_to_replace=top[:, c, 0:8], in_values=sc[:, c, :], imm_value=NEG), r=[("sc", c), ("top", c)], w=["tmp128"])
            kb.op("dve", lambda e: e.max(out=top[:, c, 8:16], in_=tmp128[:]), r=["tmp128"], w=[("top", c)])
        for h in range(8):
            c1, c2 = 2 * h, 2 * h + 1
            kb.op("dve", lambda e: e.tensor_tensor(out=cand[:].rearrange("p (a b) -> p a b", b=16), in0=top[:, c1, :].unsqueeze(2).broadcast_to([128, 16, 16]),
                                                   in1=top[:, c2, :].unsqueeze(1).broadcast_to([128, 16, 16]), op=ALU.add), r=[("top", c1), ("top", c2)], w=["cand"])
            kb.op("dve", lambda e: e.max(out=c16[:, 0:8], in_=cand[:]), r=["cand"], w=["c16"])
            kb.op("dve", lambda e: e.match_replace(out=cand2[:], in_to_replace=c16[:, 0:8], in_values=cand[:], imm_value=NEG), r=["cand", "c16"], w=["cand2"])
            kb.op("dve", lambda e: e.max(out=c16[:, 8:16], in_=cand2[:]), r=["cand2"], w=["c16"])
            kb.op("dve", lambda e: e.tensor_scalar(out=sm[:, h, 0:1], in0=c16[:, 0:1], scalar1=-1.0, scalar2=None, op0=ALU.mult), r=["c16"], w=[("sm", h)])
            kb.op("act", lambda e: e.activation(out=e16[:], in_=c16[:], func=AF.Exp, bias=sm[:, h, 0:1]), r=["c16", ("sm", h)], w=["e16"])
            kb.op("dve", lambda e: e.tensor_reduce(out=sm[:, h, 1:2], in_=e16[:], axis=AX.X, op=ALU.add), r=["e16"], w=[("sm", h)])
            kb.op("dve", lambda e: e.reciprocal(out=sm[:, h, 2:3], in_=sm[:, h, 1:2]), r=[("sm", h)], w=[("sm", h)])
            kb.op("dve", lambda e: e.tensor_scalar(out=sm[:, h, 3:4], in0=top[:, c1, 0:1], scalar1=-1.0, scalar2=None, op0=ALU.mult), r=[("top", c1)], w=[("sm", h)])
            kb.op("dve", lambda e: e.tensor_scalar(out=sm[:, h, 4:5], in0=top[:, c2, 0:1], scalar1=-1.0, scalar2=None, op0=ALU.mult), r=[("top", c2)], w=[("sm", h)])
            kb.op("dve", lambda e: e.tensor_scalar(out=a1[:, h, :], in0=sc[:, c1, :], scalar1=c16[:, 15:16], scalar2=None, op0=ALU.subtract), r=[("sc", c1), "c16"], w=[("a1", h)])
            kb.op("act", lambda e: e.activation(out=P1[:, h, :], in_=sc[:, c1, :], func=AF.Exp, bias=sm[:, h, 3:4]), r=[("sc", c1), ("sm", h)], w=[("P1", h)])
            kb.op("dve", lambda e: e.tensor_scalar(out=P1[:, h, :], in0=P1[:, h, :], scalar1=sm[:, h, 2:3], scalar2=None, op0=ALU.mult), r=[("P1", h), ("sm", h)], w=[("P1", h)])
            kb.op("act", lambda e: e.activation(out=P2[:, h, :], in_=sc[:, c2, :], func=AF.Exp, bias=sm[:, h, 4:5]), r=[("sc", c2), ("sm", h)], w=[("P2", h)])
        hk = [(nm, h) for nm in ("a1", "P1", "P2") for h in range(8)] + [("sc", c) for c in range(16)]
        for eg in range(4):
            i0 = eg * 32
            for h in range(8):
                c2 = 2 * h + 1
                kb.op("dve", lambda e: e.tensor_tensor(out=sumb[:], in0=a1[:, h, i0:i0 + 32].unsqueeze(2).broadcast_to([128, 32, 128]),
                                                       in1=sc[:, c2, :].unsqueeze(1).broadcast_to([128, 32, 128]), op=ALU.add), r=hk, w=["sumb"])
                kb.op("pool", lambda e: e.tensor_tensor(out=ppb[:], in0=P1[:, h, i0:i0 + 32].unsqueeze(2).broadcast_to([128, 32, 128]),
                                                        in1=P2[:, h, :].unsqueeze(1).broadcast_to([128, 32, 128]), op=ALU.mult), r=hk, w=["ppb"])
                if h == 0:
                    kb.op("dve", lambda e: e.scalar_tensor_tensor(out=wacc[:], in0=sumb[:], scalar=0.0, in1=ppb[:], op0=ALU.is_ge, op1=ALU.mult), r=["sumb", "ppb"], w=["wacc"])
                else:
                    kb.op("dve", lambda e: e.scalar_tensor_tensor(out=sumb[:], in0=sumb[:], scalar=0.0, in1=ppb[:], op0=ALU.is_ge, op1=ALU.mult), r=["sumb", "ppb"], w=["sumb"])
                    kb.op("dve", lambda e: e.tensor_tensor(out=wacc[:], in0=wacc[:], in1=sumb[:], op=ALU.add), r=["wacc", "sumb"], w=["wacc"])
            waf = wacc[:].rearrange("p a b -> p (a b)")
            for sg in range(8):
                e0 = eg * 4096 + sg * 512
                u_ = ub[0]
                uk = ("ub", 0)
                kb.dma("pool", u_[:], uv[:, :, e0:e0 + 512], w=[uk])
                pa = ps_act[nu % 2]
                pk = ("ps_act", nu % 2)
                nu += 1
                for kc in range(16):
                    kb.op("pe", lambda e: e.matmul(pa[:], lhsT=hT[:, kc, :], rhs=u_[:, kc, :], start=(kc == 0), stop=(kc == 15)), r=[uk] + hkeys, w=[pk])
                kb.op("act", lambda e: e.activation(out=ga[:], in_=pa[:], func=AF.Gelu), r=[pk], w=["ga"])
                kb.op("dve", lambda e: e.tensor_tensor(out=wtok[:], in0=ga[:], in1=waf[:, sg * 512:(sg + 1) * 512], op=ALU.mult), r=["ga", "wacc"], w=["wtok"])
                for q in range(4):
                    kb.op("pe", lambda e: e.transpose(ps_tr[:, q, :], wtok[:, q * 128:(q + 1) * 128], ident[:]), r=["wtok", "ident"], w=["ps_tr"])
                kb.op("act", lambda e: e.activation(out=wT[:], in_=ps_tr[:], func=AF.Copy), r=["ps_tr"], w=["wT"])
                for dg in range(4):
                    vb_ = vbuf[nv % 2]
                    vk = ("vbuf", nv % 2)
                    nv += 1
                    kb.dma("pool", vb_[:], v_d[e0:e0 + 512, dg * 512:(dg + 1) * 512].rearrange("(q p) n -> p q n", p=128), w=[vk])
                    for q in range(4):
                        first = (eg == 0 and sg == 0 and q == 0)
                        last = (eg == 3 and sg == 7 and q == 3)
                        kb.op("pe", lambda e: e.matmul(ps_o[dg][:], lhsT=wT[:, q, :], rhs=vb_[:, q, :], start=first, stop=last), r=["wT", vk], w=[("ps_o", dg)])
        kb.dma("sp", xt[:], xtok_d[tsl, :], w=["xt"])
        kb.dma("sp", gfr[:], gf_d[1 if isctx else 0], w=["gfr"])
        for dg in range(4):
            dsl = slice(dg * 512, (dg + 1) * 512)
            kb.op("dve", lambda e: e.tensor_tensor(out=gfr[:, dsl], in0=ps_o[dg][:], in1=gfr[:, dsl], op=ALU.mult), r=[("ps_o", dg), "gfr"], w=["gfr"])
        kb.op("dve", lambda e: e.tensor_tensor(out=xt[:], in0=xt[:], in1=gfr[:], op=ALU.add), r=["xt", "gfr"], w=["xt"])
        kb.op("act", lambda e: e.activation(out=sq, in_=xt[:], func=AF.Square, accum_out=st8[:, 0:1]), r=["xt", "sumb"], w=["sumb", "st8"])
        kb.op("dve", lambda e: e.tensor_scalar(out=st8[:, 1:2], in0=st8[:, 0:1], scalar1=1.0 / D, scalar2=EPS, op0=ALU.mult, op1=ALU.add), r=["st8"], w=["st8"])
        kb.op("act", lambda e: e.activation(out=st8[:, 1:2], in_=st8[:, 1:2], func=AF.Sqrt), r=["st8"], w=["st8"])
        kb.op("dve", lambda e: e.reciprocal(out=st8[:, 2:3], in_=st8[:, 1:2]), r=["st8"], w=["st8"])
        kb.op("dve", lambda e: e.tensor_scalar(out=sq, in0=fnr[:], scalar1=st8[:, 2:3], scalar2=-1.0, op0=ALU.mult, op1=ALU.add), r=["fnr", "st8", "sumb"], w=["sumb"])
        kb.op("dve", lambda e: e.tensor_tensor(out=sq, in0=sq, in1=xt[:], op=ALU.mult), r=["sumb", "xt"], w=["sumb"])
        kb.op("dve", lambda e: e.scalar_tensor_tensor(out=xt[:], in0=sq, scalar=flag[:, 0:1], in1=xt[:], op0=ALU.mult, op1=ALU.add), r=["sumb", "xt", "flag"], w=["xt"])
        kb.dma("sp", out_d[tsl, :], xt[:], r=["xt"], w=["x2"])
    kb.finish(["x2"])
    return kb


_ONESF = {}


def ones_f(kb):
    if id(kb) not in _ONESF:
        t = kb.sb([128, 128], name="onesf")
        kb.op("dve", lambda e: e.memset(t[:], 1.0), w=["onesf"])
        _ONESF[id(kb)] = t
    return _ONESF[id(kb)]


def _rev(a):
    return np.concatenate([a[:CTX][::-1], a[CTX:][::-1]], axis=0)


def kernel(**inp):
    inp = {k: np.asarray(v) for k, v in inp.items()}
    mod = run_mod(inp)
    x = inp["x"].astype(np.float32).copy()
    xc = inp["ctx"].astype(np.float32).copy()
    sm, m01, ident = hgrn_consts()
    cosT, sinT = rope_tables()
    kb_proj, kb_hg, kb_mla, kb_mg, kb_pe = build_proj(), build_hgrn(), build_mla(), build_merge(), build_peer()
    depth = inp["w_in"].shape[0]
    for l in range(depth):
        last = l == depth - 1
        P_lat, P_ctx = run_proj(x, xc, mod, l, inp, kb_proj)
        Pf = np.concatenate([P_ctx, P_lat], axis=1)
        del P_ctx
        lgt = inp["hg_lb_logits"]
        maps = []
        for i in range(NCORE):
            b, hs = i // 2, i % 2
            qs, fs, vs, lgs = [], [], [], []
            for hh in range(4):
                h = hs * 4 + hh
                for d in range(2):
                    q = Pf[b][:, h * 128:(h + 1) * 128]
                    f = Pf[b][:, 1024 + d * 1024 + h * 128:1024 + d * 1024 + (h + 1) * 128]
                    v = Pf[b][:, 3072 + h * 128:3072 + (h + 1) * 128]
                    if d == 1:
                        q, f, v = _rev(q), _rev(f), _rev(v)
                    qs.append(q.T), fs.append(f.T), vs.append(v)
                    cs = slice(h * 128, (h + 1) * 128)
                    lgs.append(np.stack([lgt[0, d, cs], lgt[1, d, cs], np.full(128, float(l), np.float32), np.zeros(128, np.float32)], -1))
            maps.append({"qT": np.ascontiguousarray(np.stack(qs)), "fT": np.ascontiguousarray(np.stack(fs)), "v": np.ascontiguousarray(np.stack(vs)),
                         "lg": np.ascontiguousarray(np.stack(lgs)).astype(np.float32), "smask": sm, "m01": m01, "ident": ident})
        res = run(kb_hg, maps)
        OF = np.empty((B, 1024, LTOT), np.float32)
        OB = np.empty((B, 1024, LTOT), np.float32)
        for i in range(NCORE):
            b, hs = i // 2, i % 2
            oT = res[i]["oT"]
            for hh in range(4):
                h = hs * 4 + hh
                OF[b, h * 128:(h + 1) * 128] = oT[hh * 2]
                OB[b, h * 128:(h + 1) * 128] = _rev(oT[hh * 2 + 1].T).T
        o = 6144
        maps = []
        for i in range(NCORE):
            b, hs = i // 2, i % 2
            wuq = inp["mla_w_uq"][l].reshape(512, 16, 192)[:, hs * 8:(hs + 1) * 8]
            wukv = inp["mla_w_ukv"][l].reshape(512, 16, 256)[:, hs * 8:(hs + 1) * 8]
            maps.append({"cqT": np.ascontiguousarray(Pf[b][:, o:o + 512].T), "ckvT": np.ascontiguousarray(Pf[b][:, o + 512:o + 1024].T),
                         "krT": np.ascontiguousarray(Pf[b][:, o + 1024:o + 1088].T), "krsT": np.ascontiguousarray(Pf[b][:, IN_COLS:IN_COLS + 64].T),
                         "cosT": cosT, "sinT": sinT,
                         "nq": np.ascontiguousarray(inp["mla_q_norm"][l].reshape(4, 128).T), "nkv": np.ascontiguousarray(inp["mla_kv_norm"][l].reshape(4, 128).T),
                         "wqn": np.ascontiguousarray(wuq[:, :, :128].reshape(512, 1024)), "wqr": np.ascontiguousarray(wuq[:, :, 128:].reshape(512, 512)),
                         "wqs": np.ascontiguousarray(wuq[:, :, 128:][:, :, ROPE_PERM].reshape(512, 512)),
                         "wkn": np.ascontiguousarray(wukv[:, :, :128].reshape(512, 1024)), "wvv": np.ascontiguousarray(wukv[:, :, 128:].reshape(512, 1024))})
        res = run(kb_mla, maps)
        ATT = np.empty((B, LTOT, D), np.float32)
        for i in range(NCORE):
            b, hs = i // 2, i % 2
            ATT[b][:, hs * 1024:(hs + 1) * 1024] = res[i]["att"]
        maps = []
        wins = (2, 4, 8, 16)
        for i in range(NCORE):
            b, hf = i // 2, i % 2
            idx = np.concatenate([CTX + hf * 2048 + np.arange(2048), hf * 128 + np.arange(128)])
            ul = np.zeros((SEQ + 2 * PH, 1024), np.float32)
            ul[PH:PH + SEQ] = P_lat[b][:, 5120:6144]
            ucx = np.zeros((CTX + 2 * PH, 1024), np.float32)
            ucx[PH:PH + CTX] = Pf[b][:CTX, 5120:6144]
            uT = np.concatenate([ul[hf * 2048:hf * 2048 + 2048 + 2 * PH], ucx[hf * 128:hf * 128 + 128 + 2 * PH]], axis=0).T
            icnt = np.empty((4, T_CORE), np.float32)
            tl_ = hf * 2048 + np.arange(2048)
            tc_ = hf * 128 + np.arange(128)
            for gi, w_ in enumerate(wins):
                icnt[gi, :2048] = 1.0 / (np.minimum(tl_ + w_ // 2, SEQ) - np.maximum(tl_ - w_ // 2, 0))
                icnt[gi, 2048:] = 1.0 / (np.minimum(tc_ + w_ // 2, CTX) - np.maximum(tc_ - w_ // 2, 0))
            maps.append({"xT": tokens_of_core(x, xc, i), "oF": np.ascontiguousarray(OF[b][:, idx]), "oB": np.ascontiguousarray(OB[b][:, idx]),
                         "gT": np.ascontiguousarray(Pf[b][idx, 4096:5120].T), "uT": np.ascontiguousarray(uT),
                         "attT": np.ascontiguousarray(ATT[b][idx].T), "grT": np.ascontiguousarray(Pf[b][idx, 7232:IN_COLS].T),
                         "hn": np.ascontiguousarray(inp["hg_norm"][l].reshape(128, 1)), "psc": np.ascontiguousarray(inp["pool_scale"][l].reshape(8, 128).T),
                         "icnt": np.ascontiguousarray(np.broadcast_to(icnt, (128, 4, T_CORE))), "pw": inp["pool_w"][l],
                         "wa": inp["w_branch_a"][l], "wb": inp["w_branch_b"][l], "wc": inp["w_branch_c"][l], "wo": inp["w_out"][l],
                         "gm": np.ascontiguousarray(np.stack([fm16(mod[b, l, 2]), fm16(mod[4, l, 2])], -1))})
        del Pf, P_lat
        res = run(kb_mg, maps)
        x1T = [res[i]["x1T"] for i in range(NCORE)]
        uT_tab = np.ascontiguousarray(inp["peer_u"][l].T)
        keysT = np.ascontiguousarray(inp["peer_keys"][l].transpose(1, 0, 3, 2).reshape(16, 128, 128))
        fnw = np.ascontiguousarray(np.broadcast_to(inp["final_norm"], (128, D)))
        flag = np.full((128, 1), 1.0 if last else 0.0, np.float32)
        maps = []
        for i in range(NCORE):
            b = i // 2
            maps.append({"xT": x1T[i], "xtok": np.ascontiguousarray(x1T[i].T), "nw": fm16(inp["norm_ffn"][l]),
                         "mv": np.ascontiguousarray(np.stack([fm16(mod[b, l, 3]), fm16(mod[b, l, 4]), fm16(mod[4, l, 3]), fm16(mod[4, l, 4])], -1)),
                         "wq": inp["peer_wq"][l], "keysT": keysT, "uT": uT_tab, "vtab": inp["peer_v"][l],
                         "gf": np.ascontiguousarray(np.stack([np.broadcast_to(mod[b, l, 5], (128, D)), np.broadcast_to(mod[4, l, 5], (128, D))])),
                         "fnw": fnw, "flag": flag})
        res = run(kb_pe, maps)
        for i in range(NCORE):
            b, hf = i // 2, i % 2
            o2 = res[i]["x2"]
            x[b, hf * 2048:(hf + 1) * 2048] = o2[:2048]
            xc[b, hf * 128:(hf + 1) * 128] = o2[2048:]
    return x.astype(np.float32)
```
